# Optimizing a Trainium2 kernel written in Bass

```python
import math
import jax, jax.numpy as jnp
from jax import lax
import numpy as np

D_MODEL = 2048
BATCH = 1
SEQ = 8192
DEPTH = 1

NORM_EPS = 1e-6
SSD_HEADS = 32
SSD_HEAD_DIM = 64
SSD_INNER = SSD_HEADS * SSD_HEAD_DIM
SSD_GROUPS = 4
SSD_STATE = 128
SSD_CONV = 4
SSD_CHUNK = 128
SSD_XBC = SSD_INNER + 2 * SSD_GROUPS * SSD_STATE
GLA_HEADS = 4
GLA_KEY_DIM = D_MODEL // 2
GLA_VAL_DIM = D_MODEL
GLA_HEAD_K = GLA_KEY_DIM // GLA_HEADS
GLA_HEAD_V = GLA_VAL_DIM // GLA_HEADS
GLA_GATE_RANK = 16
GLA_GATE_NORM = 16.0
GLA_CHUNK = 64
IN_SPLITS = (SSD_INNER, SSD_XBC, SSD_HEADS,
             GLA_KEY_DIM, GLA_KEY_DIM, GLA_VAL_DIM,
             GLA_VAL_DIM, GLA_GATE_RANK,
             SSD_INNER, GLA_VAL_DIM)
IN_DIM = SSD_INNER + SSD_XBC + SSD_HEADS + 2 * GLA_KEY_DIM + 2 * GLA_VAL_DIM + GLA_GATE_RANK + SSD_INNER + GLA_VAL_DIM
N_EXPERTS = 64
TOP_K = 8
N_EXPERT_GROUPS = 8
TOPK_GROUPS = 4
EXPERT_DIM = 512
SHARED_DIM = 512
ROUTED_SCALE = 2.5
MOE_BLOCK = 128

kernel_name = "hybrid_ssd_gla_moe_adaln_block"


def rms_norm(x, g):
    xf = x.astype(jnp.float32)
    y = xf * lax.rsqrt(jnp.mean(xf * xf, axis=-1, keepdims=True) + NORM_EPS)
    return (y * g.astype(jnp.float32)).astype(x.dtype)


def gated_group_rms_norm(y, z, g):
    yf = y.astype(jnp.float32) * jax.nn.silu(z.astype(jnp.float32))
    yg = yf.reshape(*yf.shape[:-1], SSD_GROUPS, -1)
    yg = yg * lax.rsqrt(jnp.mean(yg * yg, axis=-1, keepdims=True) + NORM_EPS)
    return (yg.reshape(yf.shape) * g.astype(jnp.float32)).astype(z.dtype)


def causal_depthwise_conv(x, w, b):
    ch = x.shape[-1]
    out = lax.conv_general_dilated(x, w[:, None, :].astype(x.dtype), window_strides=(1,),
                                   padding=[(SSD_CONV - 1, 0)],
                                   dimension_numbers=("NWC", "WIO", "NWC"),
                                   feature_group_count=ch)
    return out + b.astype(x.dtype)


def ssd_chunked(xh, dt, a, bmat, cmat):
    b, s = xh.shape[:2]
    L = SSD_CHUNK
    nc = s // L
    g = SSD_GROUPS
    r = SSD_HEADS // g
    xdt = (xh * dt[..., None]).reshape(b, nc, L, g, r, SSD_HEAD_DIM)
    acum = jnp.cumsum((dt * a).reshape(b, nc, L, g, r), axis=2)
    bc = bmat.reshape(b, nc, L, g, SSD_STATE)
    cc = cmat.reshape(b, nc, L, g, SSD_STATE)
    causal = jnp.tril(jnp.ones((L, L), dtype=bool))
    seg = acum[:, :, :, None] - acum[:, :, None]
    decay = jnp.exp(jnp.where(causal[:, :, None, None], seg, -jnp.inf))
    cb = jnp.einsum("bclgn,bcsgn->bclsg", cc, bc)
    y_diag = jnp.einsum("bclsgr,bcsgrp->bclgrp", cb[..., None] * decay, xdt)
    decay_to_end = jnp.exp(acum[:, :, -1:] - acum)
    chunk_states = jnp.einsum("bclgn,bclgrp->bcgrpn", bc, xdt * decay_to_end[..., None])
    chunk_decay = jnp.exp(acum[:, :, -1])

    def step(state, inp):
        st_c, dec_c = inp
        return state * dec_c[..., None, None] + st_c, state

    init = jnp.zeros((b, g, r, SSD_HEAD_DIM, SSD_STATE), jnp.float32)
    _, prev = lax.scan(step, init, (jnp.moveaxis(chunk_states, 1, 0), jnp.moveaxis(chunk_decay, 1, 0)))
    prev = jnp.moveaxis(prev, 0, 1)
    y_off = jnp.einsum("bclgn,bcgrpn->bclgrp", cc, prev) * jnp.exp(acum)[..., None]
    return (y_diag + y_off).reshape(b, s, SSD_HEADS, SSD_HEAD_DIM)


def gla_chunked(q, k, v, gk):
    b, s = q.shape[:2]
    L = GLA_CHUNK
    nc = s // L
    shp_k = (b, nc, L, GLA_HEADS, GLA_HEAD_K)
    q = q.reshape(shp_k) * (GLA_HEAD_K ** -0.5)
    k = k.reshape(shp_k)
    v = v.reshape(b, nc, L, GLA_HEADS, GLA_HEAD_V)
    bcum = jnp.cumsum(gk.reshape(shp_k), axis=2)
    bmid = bcum[:, :, L // 2:L // 2 + 1]
    q_rel = q * jnp.exp(bcum - bmid)
    k_rel = k * jnp.exp(bmid - bcum)
    causal = jnp.tril(jnp.ones((L, L), dtype=bool))
    att = jnp.where(causal, jnp.einsum("bclhd,bcshd->bchls", q_rel, k_rel), 0.0)
    o_intra = jnp.einsum("bchls,bcshv->bclhv", att, v)
    q_inter = q * jnp.exp(bcum)
    k_end = k * jnp.exp(bcum[:, :, -1:] - bcum)
    chunk_decay = jnp.exp(bcum[:, :, -1])

    def step(S, inp):
        qc, kc, vc, dc = inp
        o = jnp.einsum("blhd,bhdv->blhv", qc, S)
        S = S * dc[..., None] + jnp.einsum("blhd,blhv->bhdv", kc, vc)
        return S, o

    init = jnp.zeros((b, GLA_HEADS, GLA_HEAD_K, GLA_HEAD_V), jnp.float32)
    _, o_inter = lax.scan(step, init, (jnp.moveaxis(q_inter, 1, 0), jnp.moveaxis(k_end, 1, 0),
                                       jnp.moveaxis(v, 1, 0), jnp.moveaxis(chunk_decay, 1, 0)))
    o = o_intra + jnp.moveaxis(o_inter, 0, 1)
    return o.reshape(b, s, GLA_HEADS, GLA_HEAD_V)


def token_mixer(h, w_in, conv_w, conv_b, dt_bias, a_log, d_skip, ssd_norm_g,
                gla_w_gate, gla_b_gate, gla_norm_g, w_out):
    b, s, _ = h.shape
    f32 = jnp.float32
    offsets = np.cumsum(IN_SPLITS)[:-1].tolist()
    proj = h @ w_in
    z, xbc, dt_raw, q, k, v, r, g_lr, m_a, m_b = jnp.split(proj, offsets, axis=-1)
    xbc = jax.nn.silu(causal_depthwise_conv(xbc, conv_w, conv_b))
    xs, bm, cm = jnp.split(xbc, [SSD_INNER, SSD_INNER + SSD_GROUPS * SSD_STATE], axis=-1)
    dt = jax.nn.softplus(dt_raw.astype(f32) + dt_bias.astype(f32))
    a = -jnp.exp(a_log.astype(f32))
    xh = xs.reshape(b, s, SSD_HEADS, SSD_HEAD_DIM).astype(f32)
    y = ssd_chunked(xh, dt, a,
                    bm.reshape(b, s, SSD_GROUPS, SSD_STATE).astype(f32),
                    cm.reshape(b, s, SSD_GROUPS, SSD_STATE).astype(f32))
    y = y + d_skip.astype(f32)[:, None] * xh
    y_ssd = gated_group_rms_norm(y.reshape(b, s, SSD_INNER), z, ssd_norm_g)
    gk = jax.nn.log_sigmoid((g_lr @ gla_w_gate + gla_b_gate).astype(f32)) / GLA_GATE_NORM
    o = gla_chunked(q.reshape(b, s, GLA_HEADS, GLA_HEAD_K).astype(f32),
                    k.reshape(b, s, GLA_HEADS, GLA_HEAD_K).astype(f32),
                    v.reshape(b, s, GLA_HEADS, GLA_HEAD_V).astype(f32),
                    gk.reshape(b, s, GLA_HEADS, GLA_HEAD_K))
    o = rms_norm(o, gla_norm_g).astype(h.dtype) * jax.nn.silu(r.reshape(b, s, GLA_HEADS, GLA_HEAD_V))
    y_gla = o.reshape(b, s, GLA_VAL_DIM)
    mixed = jax.nn.sigmoid(m_a) * y_ssd + jax.nn.sigmoid(m_b) * y_gla
    return mixed @ w_out


def route(h2d, w_router, router_bias):
    T = h2d.shape[0]
    scores = jax.nn.sigmoid((h2d @ w_router).astype(jnp.float32))
    choice = scores + router_bias.astype(jnp.float32)
    grouped = choice.reshape(T, N_EXPERT_GROUPS, N_EXPERTS // N_EXPERT_GROUPS)
    group_score = lax.top_k(grouped, 2)[0].sum(-1)
    _, top_groups = lax.top_k(group_score, TOPK_GROUPS)
    group_mask = jnp.any(top_groups[..., None] == jnp.arange(N_EXPERT_GROUPS), axis=-2)
    expert_mask = jnp.repeat(group_mask, N_EXPERTS // N_EXPERT_GROUPS, axis=-1)
    _, idx = lax.top_k(jnp.where(expert_mask, choice, -jnp.inf), TOP_K)
    w = jnp.take_along_axis(scores, idx, axis=-1)
    w = w / (w.sum(-1, keepdims=True) + 1e-20) * ROUTED_SCALE
    return idx, w


def routed_experts(h2d, idx, w, w_e_gate, w_e_up, w_e_down):
    T, D = h2d.shape
    A = T * TOP_K
    e_flat = idx.reshape(-1)
    tok_flat = jnp.repeat(jnp.arange(T, dtype=jnp.int32), TOP_K)
    w_flat = w.reshape(-1)
    order = jnp.argsort(e_flat)
    e_sorted, tok_sorted, w_sorted = e_flat[order], tok_flat[order], w_flat[order]
    counts = jnp.bincount(e_flat, length=N_EXPERTS)
    starts = jnp.cumsum(counts) - counts
    padded = (counts + MOE_BLOCK - 1) // MOE_BLOCK * MOE_BLOCK
    pends = jnp.cumsum(padded)
    pstarts = pends - padded
    dest = pstarts[e_sorted] + (jnp.arange(A) - starts[e_sorted])
    nb = -(-A // MOE_BLOCK) + N_EXPERTS
    P = nb * MOE_BLOCK
    row_tok = jnp.full((P,), T, jnp.int32).at[dest].set(tok_sorted)
    row_w = jnp.zeros((P,), w.dtype).at[dest].set(w_sorted)
    block_exp = jnp.minimum(jnp.searchsorted(pends, jnp.arange(nb) * MOE_BLOCK, side="right"),
                            N_EXPERTS - 1)
    h_pad = jnp.concatenate([h2d, jnp.zeros((1, D), h2d.dtype)], axis=0)

    def expert_block(args):
        e, toks, wb = args
        xb = h_pad[toks]
        hid = jax.nn.silu(xb @ w_e_gate[e]) * (xb @ w_e_up[e])
        return ((hid @ w_e_down[e]) * wb[:, None]).astype(h2d.dtype)

    out = lax.map(expert_block, (block_exp, row_tok.reshape(nb, MOE_BLOCK), row_w.reshape(nb, MOE_BLOCK)))
    return jax.ops.segment_sum(out.reshape(P, D), row_tok, num_segments=T + 1)[:T]


def moe_ffn(h, w_router, router_bias, w_e_gate, w_e_up, w_e_down, w_s_gate, w_s_up, w_s_down):
    b, s, d = h.shape
    h2d = h.reshape(b * s, d)
    idx, w = route(h2d, w_router, router_bias)
    shared = (jax.nn.silu(h2d @ w_s_gate) * (h2d @ w_s_up)) @ w_s_down
    y = routed_experts(h2d, idx, w, w_e_gate, w_e_up, w_e_down) + shared
    return y.reshape(b, s, d)


def setup_inputs(seed: int = 0) -> dict:
    key = jax.random.key(seed)
    ks = jax.random.split(key, 32)
    f32 = jnp.float32
    L = DEPTH

    def nrm(k, shape, scale):
        return jax.random.normal(k, shape, f32) * scale

    dt0 = jnp.exp(jax.random.uniform(ks[8], (L, SSD_HEADS), f32, math.log(1e-3), math.log(1e-1)))
    return {
        "x": nrm(ks[0], (BATCH, SEQ, D_MODEL), 1.0),
        "c": nrm(ks[1], (BATCH, D_MODEL), 1.0),
        "w_ada": nrm(ks[2], (L, D_MODEL, 6 * D_MODEL), 0.5 * D_MODEL ** -0.5),
        "b_ada": nrm(ks[3], (L, 6 * D_MODEL), 0.01),
        "norm1_g": 1.0 + nrm(ks[4], (L, D_MODEL), 0.02),
        "w_in": nrm(ks[5], (L, D_MODEL, IN_DIM), D_MODEL ** -0.5),
        "conv_w": nrm(ks[6], (L, SSD_CONV, SSD_XBC), SSD_CONV ** -0.5),
        "conv_b": nrm(ks[7], (L, SSD_XBC), 0.01),
        "dt_bias": dt0 + jnp.log(-jnp.expm1(-dt0)),
        "a_log": jnp.log(jax.random.uniform(ks[9], (L, SSD_HEADS), f32, 1.0, 16.0)),
        "d_skip": 1.0 + nrm(ks[10], (L, SSD_HEADS), 0.1),
        "ssd_norm_g": 1.0 + nrm(ks[11], (L, SSD_INNER), 0.02),
        "gla_w_gate": nrm(ks[12], (L, GLA_GATE_RANK, GLA_KEY_DIM), GLA_GATE_RANK ** -0.5),
        "gla_b_gate": nrm(ks[13], (L, GLA_KEY_DIM), 0.01),
        "gla_norm_g": 1.0 + nrm(ks[14], (L, GLA_HEAD_V), 0.02),
        "w_out": nrm(ks[15], (L, D_MODEL, D_MODEL), D_MODEL ** -0.5),
        "norm2_g": 1.0 + nrm(ks[16], (L, D_MODEL), 0.02),
        "w_router": nrm(ks[17], (L, D_MODEL, N_EXPERTS), D_MODEL ** -0.5),
        "router_bias": nrm(ks[18], (L, N_EXPERTS), 0.01),
        "w_e_gate": nrm(ks[19], (L, N_EXPERTS, D_MODEL, EXPERT_DIM), D_MODEL ** -0.5),
        "w_e_up": nrm(ks[20], (L, N_EXPERTS, D_MODEL, EXPERT_DIM), D_MODEL ** -0.5),
        "w_e_down": nrm(ks[21], (L, N_EXPERTS, EXPERT_DIM, D_MODEL), EXPERT_DIM ** -0.5),
        "w_s_gate": nrm(ks[22], (L, D_MODEL, SHARED_DIM), D_MODEL ** -0.5),
        "w_s_up": nrm(ks[23], (L, D_MODEL, SHARED_DIM), D_MODEL ** -0.5),
        "w_s_down": nrm(ks[24], (L, SHARED_DIM, D_MODEL), SHARED_DIM ** -0.5),
        "normf_g": 1.0 + nrm(ks[25], (D_MODEL,), 0.02),
    }


def reference(x, c, w_ada, b_ada, norm1_g, w_in, conv_w, conv_b, dt_bias, a_log, d_skip,
              ssd_norm_g, gla_w_gate, gla_b_gate, gla_norm_g, w_out, norm2_g, w_router,
              router_bias, w_e_gate, w_e_up, w_e_down, w_s_gate, w_s_up, w_s_down, normf_g):
    c_act = jax.nn.silu(c)
    for layer in range(DEPTH):
        mod = (c_act @ w_ada[layer] + b_ada[layer])[:, None, :]
        sh1, sc1, g1, sh2, sc2, g2 = jnp.split(mod, 6, axis=-1)
        h = rms_norm(x, norm1_g[layer]) * (1.0 + sc1) + sh1
        x = x + g1 * token_mixer(h, w_in[layer], conv_w[layer], conv_b[layer], dt_bias[layer],
                                 a_log[layer], d_skip[layer], ssd_norm_g[layer], gla_w_gate[layer],
                                 gla_b_gate[layer], gla_norm_g[layer], w_out[layer])
        h = rms_norm(x, norm2_g[layer]) * (1.0 + sc2) + sh2
        x = x + g2 * moe_ffn(h, w_router[layer], router_bias[layer], w_e_gate[layer], w_e_up[layer],
                             w_e_down[layer], w_s_gate[layer], w_s_up[layer], w_s_down[layer])
    return rms_norm(x, normf_g)
```

```python
import types
import numpy as np
import concourse.bass as bass
import concourse.mybir as mybir
from concourse.bass_utils import run_bass_kernel_spmd

F32 = mybir.dt.float32
BF16 = mybir.dt.bfloat16
AF = mybir.ActivationFunctionType
ALU = mybir.AluOpType
AX = mybir.AxisListType

NCORES = 8
D = 2048
T = 1024
NT = T // 128
IN_DIM = 15408
OFF_Z, OFF_X, OFF_B, OFF_C, OFF_DT = 0, 2048, 4096, 4608, 5120
OFF_Q, OFF_K, OFF_V, OFF_R, OFF_GLR, OFF_MA, OFF_MB = 5152, 6176, 7200, 9248, 11296, 11312, 13360
EPS = 1e-6
NEXP = 64

C_G1, C_G2, C_CW, C_CB, C_GB, C_FLAG, C_CT, C_NCOLS = 0, 16, 32, 128, 152, 160, 176, 192
R_GG, R_DTB, R_ALOG, R_DSK, R_RB, R_NROWS = 0, 512, 544, 576, 608, 672
K_ID, K_TRI, K_STRICT, K_GLA, K_RESET, K_N = 0, 128, 256, 384, 512, 1536
XW = 2048 + 4096 + 32 + 8


def _freeze(fn):
    if fn.__closure__ is None:
        return fn
    cells = tuple(types.CellType(c.cell_contents) for c in fn.__closure__)
    return types.FunctionType(fn.__code__, fn.__globals__, fn.__name__, fn.__defaults__, cells)


class Prog:
    ENGS = ("pe", "act", "dve", "pool", "sp")
    NSEM = {"sp": 40, "pool": 16, "act": 10, "cc": 12}

    def __init__(self, nc):
        self.nc = nc
        self.ops = []
        self.last_w = {}
        self.readers = {}
        self.sem_cnt = {}
        self.q_cnt = {}
        self.pending = {}
        self.last_op = {}

    def _add(self, eng, fn, r, w, is_async=False, inc=16, extra=()):
        op = dict(eng=eng, fn=_freeze(fn), dma=None, deps=None, signal=False, idx=len(self.ops), inc=inc)
        deps = {}
        if is_async:
            q = "cc" if inc == 1 else eng
            n = self.q_cnt.get(q, 0)
            self.q_cnt[q] = n + 1
            sk = (q, n % self.NSEM[q])
            prev = self.sem_cnt.get(sk, 0)
            self.sem_cnt[sk] = prev + 1
            op["dma"] = (sk, inc * (prev + 1))
            if prev:
                deps[("g", sk)] = inc * prev

        def dep(p, raw):
            if p is None:
                return
            if p["dma"] is not None:
                key = ("g", p["dma"][0])
                deps[key] = max(deps.get(key, 0), p["dma"][1])
                return
            if p["eng"] == eng and not is_async:
                if eng == "pe" or not raw:
                    return
            key = ("e", p["eng"])
            if key not in deps or deps[key]["idx"] < p["idx"]:
                deps[key] = p

        pb = self.pending.pop(eng, None)
        if pb is not None:
            dep(pb, True)
        for p_ in extra:
            dep(p_, True)
        for k in r:
            dep(self.last_w.get(k), True)
        for k in w:
            dep(self.last_w.get(k), False)
            for p in self.readers.get(k, {}).values():
                dep(p, False)
        op["deps"] = deps
        for key, p in deps.items():
            if key[0] == "e":
                p["signal"] = True
        for k in w:
            self.last_w[k] = op
            self.readers[k] = {}
        for k in r:
            rk = ("d", op["idx"]) if is_async else eng
            self.readers.setdefault(k, {})[rk] = op
        self.ops.append(op)
        if not is_async:
            self.last_op[eng] = op
        return op

    def coll(self, fn, r=(), w=(), group=None):
        return self._add("pool", fn, r, w, is_async=True, inc=1)

    def barrier(self, scratch):
        extra = [p for e, p in self.last_op.items() if e != "dve"]
        bop = self._add("dve", lambda e: e.memset(scratch, 0.0), (), (), extra=extra)
        for sk, c in self.sem_cnt.items():
            bop["deps"][("g", sk)] = c * (1 if sk[0] == "cc" else 16)
        bop["signal"] = True
        for e in self.ENGS:
            if e != "dve":
                self.pending[e] = bop

    def pe(self, fn, r=(), w=()):
        return self._add("pe", fn, r, w)

    def act(self, fn, r=(), w=()):
        return self._add("act", fn, r, w)

    def dve(self, fn, r=(), w=()):
        return self._add("dve", fn, r, w)

    def pool(self, fn, r=(), w=()):
        return self._add("pool", fn, r, w)

    def dma(self, queue, fn, r=(), w=(), group=None):
        return self._add(queue, fn, r, w, is_async=True)

    def emit(self, final_wait_groups=None):
        nc = self.nc
        sem = {e: nc.alloc_semaphore(f"s_{e}") for e in ("pe", "act", "dve", "pool")}
        gsem = {sk: nc.alloc_semaphore(f"g_{sk[0]}{sk[1]}") for sk in self.sem_cnt}
        cnt = {e: 0 for e in sem}
        for op in self.ops:
            if op["dma"] is None and op["signal"]:
                cnt[op["eng"]] += 1
                op["ticket"] = cnt[op["eng"]]
        engobj = {"pe": "tensor", "act": "scalar", "dve": "vector", "pool": "gpsimd", "sp": "sync"}
        with nc.Block() as block:
            for en in self.ENGS:
                mine = [op for op in self.ops if op["eng"] == en]
                if not mine:
                    continue

                def body(e, mine=mine, en=en):
                    waited = {}
                    for op in mine:
                        for key, p in op["deps"].items():
                            if key[0] == "g":
                                s, v = gsem[key[1]], p
                            else:
                                s, v = sem[key[1]], p["ticket"]
                            if waited.get(key, 0) >= v:
                                continue
                            waited[key] = v
                            e.wait_ge(s, v)
                        ins = op["fn"](e)
                        if op["dma"] is not None:
                            ins.then_inc(gsem[op["dma"][0]], op["inc"])
                        elif op["signal"]:
                            ins.then_inc(sem[en], 1)
                    if en == "sp":
                        for sk, c in self.sem_cnt.items():
                            e.wait_ge(gsem[sk], c * (1 if sk[0] == "cc" else 16))

                getattr(block, engobj[en])(body)


def build_program(upto=99, debug=False, n_groups=8):
    nc = bass.Bass("TRN2", target_bir_lowering=False)
    P = Prog(nc)
    dbg_outs = {}

    def din(name, shape, dt=F32):
        return nc.dram_tensor(name, list(shape), dt, kind="ExternalInput").ap()

    x_d = din("x", [T, D])
    xh_d = din("xh", [3, D])
    cols_d = din("cols", [128, C_NCOLS])
    rows_d = din("rows", [128, R_NROWS])
    rows2_d = din("rows2", [128, 4096])
    consts_d = din("consts", [128, K_N])
    wada_d = din("wada", [D, 1536])
    bada_d = din("bada", [1, 1536])
    if upto >= 1.5:
        win_d = din("win", [D, IN_DIM])
        wgate_d = din("wgate", [16, 1024])
    if upto >= 4.5:
        wout_d = din("wout", [D, D])
        wr_d = din("wr", [128, 16 * NEXP])
    if upto >= 7:
        if n_groups > 0:
            wexp_d = din("wexp", [8 * 6144, 512])
        wsh_d = din("wsh", [6144, 512])
        wloc = [nc.dram_tensor(f"wloc{k}", [6144, 512], F32) for k in range(n_groups)]
        gath = [nc.dram_tensor(f"gath{k}", [8 * 6144, 512], F32, addr_space="Shared") for k in range(n_groups)]
    out_d = nc.dram_tensor("out", [T, D], F32, kind="ExternalOutput").ap()

    ag1_in = nc.dram_tensor("ag1_in", [1, 1536], F32)
    ag1_out = nc.dram_tensor("ag1_out", [8, 1536], F32, addr_space="Shared")
    ag2_in = nc.dram_tensor("ag2_in", [128, XW], F32)
    ag2_out = nc.dram_tensor("ag2_out", [8 * 128, XW], F32, addr_space="Shared")
    x1_spill = nc.dram_tensor("x1_spill", [T, D], F32).ap()
    yloc_d = nc.dram_tensor("yloc_d", [T, D], F32).ap()
    oloc_d = nc.dram_tensor("oloc_d", [T, D], F32).ap()

    def dump(name, ap_sb, shape, rkeys, dt=F32):
        if not debug:
            return
        t = nc.dram_tensor("dbg_" + name, list(shape), dt, kind="ExternalOutput").ap()
        dbg_outs[name] = t
        P.dma("sp", lambda e: e.dma_start(out=t, in_=ap_sb), r=rkeys, w=(), group="dbg")

    def dump_dram(name, src, shape, rkeys):
        if not debug:
            return
        t = nc.dram_tensor("dbg_" + name, list(shape), F32, kind="ExternalOutput").ap()
        dbg_outs[name] = t
        P.dma("sp", lambda e: e.dma_start(out=t, in_=src), r=rkeys, w=(), group="dbg")

    def sb(name, shape, dt):
        return nc.alloc_sbuf_tensor("sb_" + name, list(shape), dt)

    A32 = sb("A32", [128, 16, T], BF16)
    B32 = sb("B32", [128, 16384], BF16)
    E32 = sb("E32", [128, 16384], BF16)
    NR = 4
    RING = [sb(f"ring{i}", [128, 16, 512], BF16) for i in range(NR)]
    WKN = 6600
    WK = sb("WK", [128, WKN], F32)
    consts = sb("consts", [128, K_N], F32)
    cols = sb("cols", [128, C_NCOLS], F32)
    rows = sb("rows", [128, R_NROWS], F32)
    small = sb("small", [128, 3072], F32)
    identb = sb("identb", [128, 128], BF16)
    cact = sb("cact", [128, 16], BF16)
    hTh = sb("hTh", [128, 16, 4], BF16)
    psb = [nc.alloc_psum_tensor(f"ps{i}", [128, 512], F32) for i in range(8)]

    B32f = B32[:, :].bitcast(F32)
    A32f = A32[:, :, :].rearrange("p a b -> p (a b)").bitcast(F32)
    CT = E32[:, 0:4096].rearrange("p (a b) -> p a b", a=4)
    QG = E32[:, 4096:12288].rearrange("p (a b) -> p a b", a=8)
    sparef = E32[:, 12288:16384].bitcast(F32)
    h2T = E32[:, :].rearrange("p (a b) -> p a b", a=16)
    hT = A32

    ident = consts[:, K_ID:K_ID + 128]
    tri = consts[:, K_TRI:K_TRI + 128]
    strict = consts[:, K_STRICT:K_STRICT + 128]
    glamask = consts[:, K_GLA:K_GLA + 128]
    resetm = consts[:, K_RESET:K_RESET + 1024]

    _so = [0]

    def sm(n):
        a = small[:, _so[0]:_so[0] + n]
        _so[0] += n
        assert _so[0] <= 3072
        return a

    modT = sm(96)
    A1 = sm(16); A2 = sm(16)
    dtv = sm(NT * 32); av = sm(NT * 32)
    acum = sm(NT * 32); eacum = sm(NT * 32); dte = sm(NT * 32); cdec = sm(NT * 32); eglob = sm(NT * 32)
    totrun = sm(32); Arow = sm(32)
    onesf = sm(128)
    negb = sm(8)
    Gt = sm(NT * 64)
    ssq = sm(16); rstd = sm(16)
    epsc = sm(1)
    omf = sm(8)
    dcy = sm(32)
    midv = sm(16); lastv = sm(16)
    ss1 = sm(2); ss2 = sm(2)
    rt = sm(64)
    barscr = sm(1)
    flg = cols[:, C_FLAG:C_FLAG + 16]

    def v3(ap2, a):
        return ap2.rearrange("p (a b) -> p a b", a=a)

    dt3, a3, acum3, eacum3, dte3, cdec3, eglob3 = (v3(t_, NT) for t_ in (dtv, av, acum, eacum, dte, cdec, eglob))
    Gt3 = v3(Gt, NT)

    def bc(ap2, n):
        return ap2.unsqueeze(2).to_broadcast([128, ap2.shape[1], n])

    def h8(ap2):
        return ap2.rearrange("p (h d) -> p h d", h=8)

    P.dma("sp", lambda e: e.dma_start(out=consts[:, :], in_=consts_d), w=["consts"], group="par")
    P.dma("sp", lambda e: e.dma_start(out=cols[:, :], in_=cols_d), w=["cols"], group="par")
    P.dma("sp", lambda e: e.dma_start(out=rows[:, :], in_=rows_d), w=["rows"], group="par")
    P.dve(lambda e: e.tensor_copy(out=identb[:, :], in_=ident), r=["consts"], w=["identb"])
    P.dve(lambda e: e.memset(onesf, 1.0), w=["onesf"])
    P.dve(lambda e: e.memset(epsc, EPS), w=["epsc"])

    P.act(lambda e: e.activation(out=cact[:, :], in_=cols[:, C_CT:C_CT + 16], func=AF.Silu), r=["cols"], w=["cact"])
    wada_v = wada_d.rearrange("(k p) c -> p k c", p=128)
    modrow = WK[0:1, 0:1536]
    brow = WK[0:1, 1536:3072]
    P.dma("sp", lambda e: e.dma_start(out=brow, in_=bada_d), w=["brow"], group="par")
    for b in range(3):
        P.dma("pool", lambda e, b=b: e.dma_start(out=RING[b][:, :, :], in_=wada_v[:, :, b * 512:(b + 1) * 512]),
              w=[f"ring{b}"], group=f"ring{b}")
    for b in range(3):
        for k in range(16):
            P.pe(lambda e, b=b, k=k: e.matmul(psb[b][0:1, :], lhsT=cact[:, k:k + 1], rhs=RING[b][:, k, :],
                                              start=(k == 0), stop=(k == 15)),
                 r=["cact", f"ring{b}"], w=[f"ps{b}"])
        P.dve(lambda e, b=b: e.tensor_tensor(out=modrow[:, b * 512:(b + 1) * 512], in0=psb[b][0:1, :],
                                             in1=brow[:, b * 512:(b + 1) * 512], op=ALU.add),
              r=[f"ps{b}", "brow"], w=["modrow"])
    P.dma("sp", lambda e: e.dma_start(out=ag1_in.ap(), in_=modrow), r=["modrow"], w=["ag1_in"], group="ag1i")
    P.coll(lambda e: e.collective_compute("AllGather", ALU.bypass, replica_groups=[list(range(NCORES))],
                                          ins=[ag1_in.ap()], outs=[ag1_out.ap()]),
           r=["ag1_in"], w=["ag1_out"], group="cc1")
    def weight_ag(k):
        P.coll(lambda e, k=k: e.collective_compute("AllGather", ALU.bypass, replica_groups=[list(range(NCORES))],
                                                   ins=[wloc[k].ap()], outs=[gath[k].ap()]),
               r=[("wloc", k)], w=[("gath", k)], group=f"ccw{k}")

    if upto >= 7:
        for k in range(n_groups):
            P.dma("act", lambda e, k=k: e.dma_start(out=wloc[k].ap(), in_=wexp_d[k * 6144:(k + 1) * 6144, :]),
                  w=[("wloc", k)], group="wl")
        for k in range(min(2, n_groups)):
            weight_ag(k)
    mod96 = WK[0:96, 4096:4096 + 128]
    P.dma("sp", lambda e: e.dma_start(out=mod96, in_=ag1_out.ap().rearrange("r (a p) -> (r a) p", p=128)),
          r=["ag1_out"], w=["mod96"], group="par")
    P.pe(lambda e: e.transpose(out=psb[3][:, 0:96], in_=mod96, identity=ident[0:96, 0:96]),
         r=["mod96", "consts"], w=["ps3"])
    P.act(lambda e: e.activation(out=modT, in_=psb[3][:, 0:96], func=AF.Copy), r=["ps3"], w=["modT"])
    P.dve(lambda e: e.scalar_tensor_tensor(out=A1, in0=modT[:, 16:32], scalar=1.0, in1=cols[:, C_G1:C_G1 + 16],
                                           op0=ALU.add, op1=ALU.mult), r=["modT", "cols"], w=["A1"])
    P.dve(lambda e: e.scalar_tensor_tensor(out=A2, in0=modT[:, 64:80], scalar=1.0, in1=cols[:, C_G2:C_G2 + 16],
                                           op0=ALU.add, op1=ALU.mult), r=["modT", "cols"], w=["A2"])
    ag1_flat = ag1_out.ap().rearrange("r c -> (r c)")
    dump("modT", modT, [128, 96], ["modT"])
    P.barrier(barscr)
    P.act(lambda e: e.activation(out=Arow, in_=rows[:, R_ALOG:R_ALOG + 32], func=AF.Exp), r=["rows"], w=["Arow"])
    P.dve(lambda e: e.tensor_scalar(out=Arow, in0=Arow, scalar1=-1.0, scalar2=None, op0=ALU.mult), r=["Arow"], w=["Arow"])
    P.dve(lambda e: e.tensor_scalar(out=negb, in0=cols[:, C_GB:C_GB + 8], scalar1=-1.0, scalar2=None, op0=ALU.mult),
          r=["cols"], w=["negb"])
    P.dve(lambda e: e.tensor_scalar(out=omf, in0=flg[:, 1:9], scalar1=-1.0, scalar2=1.0, op0=ALU.mult, op1=ALU.add),
          r=["cols"], w=["omf"])
    dump("Arow0", Arow, [128, 32], ["Arow"])

    def norm_tile(i, src_ap, np_, A_, Bcol0, dstT, dst_cols, xkey, xs_, junk_, wkey, f32dst=None, jkey="junk"):
        b = i % 2
        P.act(lambda e: e.activation(out=junk_[0:np_, :], in_=src_ap, func=AF.Square, accum_out=ssq[0:np_, i:i + 1]),
              r=[xkey], w=[jkey, f"ssq{i}"])
        P.act(lambda e: e.activation(out=rstd[0:np_, i:i + 1], in_=ssq[0:np_, i:i + 1], func=AF.Sqrt,
                                     scale=1.0 / D, bias=epsc[0:np_, :]),
              r=[f"ssq{i}", "epsc"], w=[f"rstd{i}"])
        P.dve(lambda e: e.reciprocal(out=rstd[0:np_, i:i + 1], in_=rstd[0:np_, i:i + 1]), r=[f"rstd{i}"], w=[f"rstd{i}"])
        P.dve(lambda e: e.tensor_scalar(out=xs_[b][0:np_, :], in0=src_ap, scalar1=rstd[0:np_, i:i + 1], scalar2=None,
                                        op0=ALU.mult), r=[xkey, f"rstd{i}"], w=[f"xs{b}"])
        for q4 in range(4):
            pb = 4 + (q4 % 2) + 2 * b
            for kk in range(4):
                k = q4 * 4 + kk
                P.pe(lambda e, k=k, kk=kk, pb=pb: e.transpose(out=psb[pb][:, kk * 128:kk * 128 + np_],
                                                             in_=xs_[b][0:np_, k * 128:(k + 1) * 128],
                                                             identity=ident[0:np_, 0:np_]),
                     r=[f"xs{b}", "consts"], w=[f"ps{pb}"])
            for kk in range(4):
                k = q4 * 4 + kk
                if kk % 2 == 0 or f32dst is not None:
                    P.act(lambda e, k=k, kk=kk, pb=pb: e.activation(
                        out=dstT[:, k, dst_cols], in_=psb[pb][:, kk * 128:kk * 128 + np_], func=AF.Identity,
                        scale=A_[:, k:k + 1], bias=modT[:, Bcol0 + k:Bcol0 + k + 1]),
                        r=[f"ps{pb}", "A1", "A2", "modT"], w=[wkey])
                else:
                    P.dve(lambda e, k=k, kk=kk, pb=pb: e.tensor_scalar(
                        out=dstT[:, k, dst_cols], in0=psb[pb][:, kk * 128:kk * 128 + np_],
                        scalar1=A_[:, k:k + 1], scalar2=modT[:, Bcol0 + k:Bcol0 + k + 1], op0=ALU.mult, op1=ALU.add),
                        r=[f"ps{pb}", "A1", "A2", "modT"], w=[wkey])
                if f32dst is not None:
                    P.act(lambda e, k=k, kk=kk, pb=pb: e.activation(
                        out=f32dst[:, k, :], in_=psb[pb][:, kk * 128:kk * 128 + np_], func=AF.Identity,
                        scale=A_[:, k:k + 1], bias=modT[:, Bcol0 + k:Bcol0 + k + 1]),
                        r=[f"ps{pb}", "A1", "A2", "modT"], w=["h2f"])

    xt = [WK[:, 0:2048], WK[:, 2048:4096]]
    xs1 = [WK[:, 4096:6144], B32f[:, 0:2048]]
    junk1 = B32f[:, 2048:4096]
    hkeys = [("hT", i) for i in range(NT)]
    hhkey = ("hTh",)
    for i in range(NT):
        b = i % 2
        P.dma("sp", lambda e, i=i, b=b: e.dma_start(out=xt[b], in_=x_d[i * 128:(i + 1) * 128, :]),
              w=[f"xt{b}"], group=f"xt{b}")
        norm_tile(i, xt[b], 128, A1, 0, hT, slice(i * 128, (i + 1) * 128), f"xt{b}", xs1, junk1, hkeys[i])
    P.dma("sp", lambda e: e.dma_start(out=xt[0][0:3, :], in_=xh_d), w=["xt0"], group="xt0")
    norm_tile(NT, xt[0][0:3, :], 3, A1, 0, hTh, slice(0, 3), "xt0", xs1, junk1, hhkey)
    dump("hT", hT[:, :, :], [128, 16, T], hkeys, BF16)
    if upto == 0.9:
        return finish(nc, P, dbg_outs)
    P.barrier(barscr)
    if upto <= 1:
        return finish(nc, P, dbg_outs)

    ring_ctr = [3]

    def load_w(src_ap, rk=()):
        s = ring_ctr[0] % NR
        ring_ctr[0] += 1
        a, bb = src_ap.shape[1], src_ap.shape[2]
        view = RING[s][:, :, :].rearrange("p a b -> p (a b)")[:, 0:a * bb].rearrange("p (a b) -> p a b", a=a)
        if bb > 1024:
            for c0 in range(0, bb, 1024):
                P.dma("pool", lambda e, c0=c0: e.dma_start(out=view[:, :, c0:c0 + 1024], in_=src_ap[:, :, c0:c0 + 1024]),
                      r=list(rk), w=[f"ring{s}"], group=f"ring{s}")
        else:
            P.dma("pool", lambda e: e.dma_start(out=view, in_=src_ap), r=list(rk), w=[f"ring{s}"], group=f"ring{s}")
        return view, f"ring{s}"

    win_v = win_d.rearrange("(k p) c -> p k c", p=128)

    wdt, wdt_k = load_w(win_v[:, :, OFF_DT:OFF_DT + 32])
    for i in range(NT):
        pb = i % 2
        for k in range(16):
            P.pe(lambda e, i=i, k=k, pb=pb: e.matmul(psb[pb][:, 0:32], lhsT=hT[:, k, i * 128:(i + 1) * 128],
                                                      rhs=wdt[:, k, :], start=(k == 0), stop=(k == 15)),
                 r=[hkeys[i], wdt_k], w=[f"ps{pb}"])
        P.dve(lambda e, i=i, pb=pb: e.tensor_tensor(out=dt3[:, i, :], in0=psb[pb][:, 0:32],
                                                    in1=rows[:, R_DTB:R_DTB + 32], op=ALU.add),
              r=[f"ps{pb}", "rows"], w=["dtall"])
    P.act(lambda e: e.activation(out=dtv, in_=dtv, func=AF.Exp), r=["dtall"], w=["dtall"])
    P.act(lambda e: e.activation(out=dtv, in_=dtv, func=AF.Ln, bias=1.0), r=["dtall"], w=["dtall"])
    for i in range(NT):
        P.dve(lambda e, i=i: e.tensor_tensor(out=a3[:, i, :], in0=dt3[:, i, :], in1=Arow, op=ALU.mult),
              r=["dtall", "Arow"], w=[f"a{i}"])
    P.dve(lambda e: e.memset(totrun, 0.0), w=["totrun"])
    for i in range(NT):
        P.pe(lambda e, i=i: e.matmul(psb[2][:, 0:32], lhsT=tri, rhs=a3[:, i, :], start=True, stop=True),
             r=["consts", f"a{i}"], w=["ps2"])
        P.pe(lambda e, i=i: e.matmul(psb[3][:, 0:32], lhsT=onesf, rhs=a3[:, i, :], start=True, stop=True),
             r=["onesf", f"a{i}"], w=["ps3"])
        P.act(lambda e, i=i: e.activation(out=acum3[:, i, :], in_=psb[2][:, 0:32], func=AF.Copy), r=["ps2"], w=[f"acum{i}"])
        P.act(lambda e, i=i: e.activation(out=cdec3[:, i, :], in_=psb[3][:, 0:32], func=AF.Exp), r=["ps3"], w=[f"cdec{i}"])
        P.dve(lambda e, i=i: e.tensor_tensor(out=dte3[:, i, :], in0=psb[3][:, 0:32], in1=acum3[:, i, :], op=ALU.subtract),
              r=["ps3", f"acum{i}"], w=[f"dte{i}"])
        P.act(lambda e, i=i: e.activation(out=dte3[:, i, :], in_=dte3[:, i, :], func=AF.Exp), r=[f"dte{i}"], w=[f"dte{i}"])
        P.act(lambda e, i=i: e.activation(out=eacum3[:, i, :], in_=acum3[:, i, :], func=AF.Exp), r=[f"acum{i}"], w=[f"eacum{i}"])
        P.dve(lambda e, i=i: e.tensor_tensor(out=eglob3[:, i, :], in0=acum3[:, i, :], in1=totrun, op=ALU.add),
              r=[f"acum{i}", "totrun"], w=[f"eglob{i}"])
        P.act(lambda e, i=i: e.activation(out=eglob3[:, i, :], in_=eglob3[:, i, :], func=AF.Exp), r=[f"eglob{i}"], w=[f"eglob{i}"])
        P.dve(lambda e, i=i: e.tensor_tensor(out=totrun, in0=psb[3][:, 0:32], in1=totrun, op=ALU.add),
              r=["ps3", "totrun"], w=["totrun"])
    dtot = rt[:, 0:32]
    P.act(lambda e: e.activation(out=dtot, in_=totrun, func=AF.Exp), r=["totrun"], w=["dtot"])
    P.dma("sp", lambda e: e.dma_start(out=ag2_in.ap()[:, 6144:6176], in_=dtot), r=["dtot"], w=["ag2_in"], group="ag2i")
    dump("dt", dtv, [128, NT * 32], ["dtall"])
    dump("eglob", eglob, [128, NT * 32], [f"eglob{i}" for i in range(NT)])

    pre = WK[:, 0:6 * 1028].rearrange("p (a b) -> p a b", a=6)
    CBm = WK[:, 6168:6296]
    Lh = [WK[:, 6296:6424], WK[:, 6424:6552]]
    xTc = B32[:, 0:4096].rearrange("p (a b) -> p a b", a=4)
    BTc = B32[:, 4096:5120]
    xtok = B32[:, 5120:5632]
    xdt = B32[:, 5632:6144]
    xdte = B32[:, 6144:6656]
    Btok = B32[:, 6656:6784]
    Mh = B32[:, 6784:7808]
    Sbf = B32[:, 7808:8320]
    dec = B32f[:, 4160:4672]
    ytmp = B32f[:, 4672:5184]
    cacc = B32f[:, 5184:6208]
    ybuf = [B32f[:, 6208:6720], B32f[:, 6720:7232]]
    Sst = B32f[:, 7232:7744]

    for g in range(4):
        wx, wx_k = load_w(win_v[:, :, OFF_X + g * 512:OFF_X + (g + 1) * 512])
        wb_, wb_k = load_w(win_v[:, :, OFF_B + g * 128:OFF_B + (g + 1) * 128])
        wc_, wc_k = load_w(win_v[:, :, OFF_C + g * 128:OFF_C + (g + 1) * 128])
        for cc in range(6):
            if cc < 4:
                wv, wk_ = wx[:, :, cc * 128:(cc + 1) * 128], wx_k
            elif cc == 4:
                wv, wk_ = wb_, wb_k
            else:
                wv, wk_ = wc_, wc_k
            for half in range(2):
                pb = (cc * 2 + half) % 4
                for k in range(16):
                    P.pe(lambda e, wv=wv, k=k, half=half, pb=pb: e.matmul(
                        psb[pb][:, :], lhsT=wv[:, k, :], rhs=hT[:, k, half * 512:(half + 1) * 512],
                        start=(k == 0), stop=(k == 15)),
                        r=[wk_] + hkeys[half * 4:(half + 1) * 4], w=[f"ps{pb}"])
                if half == 0:
                    P.act(lambda e, cc=cc, pb=pb: e.activation(out=pre[:, cc, 3:515], in_=psb[pb][:, :], func=AF.Copy),
                          r=[f"ps{pb}"], w=[f"pre{cc}"])
                else:
                    P.dve(lambda e, cc=cc, pb=pb: e.tensor_copy(out=pre[:, cc, 515:1027], in_=psb[pb][:, :]),
                          r=[f"ps{pb}"], w=[f"pre{cc}"])
            for k in range(16):
                P.pe(lambda e, wv=wv, k=k: e.matmul(psb[4][:, 0:3], lhsT=wv[:, k, :], rhs=hTh[:, k, 0:3],
                                                    start=(k == 0), stop=(k == 15)),
                     r=[wk_, hhkey], w=["ps4"])
            P.dve(lambda e, cc=cc: e.tensor_scalar(out=pre[:, cc, 0:3], in0=psb[4][:, 0:3], scalar1=flg[:, 0:1],
                                                   scalar2=None, op0=ALU.mult), r=["ps4", "cols"], w=[f"pre{cc}"])
            gc = (g * 4 + cc) if cc < 4 else (16 + g if cc == 4 else 20 + g)
            cw = cols[:, C_CW + gc * 4:C_CW + gc * 4 + 4]
            cb = cols[:, C_CB + gc:C_CB + gc + 1]
            P.dve(lambda e, cc=cc, cw=cw, cb=cb: e.tensor_scalar(out=cacc, in0=pre[:, cc, 0:1024], scalar1=cw[:, 0:1],
                                                               scalar2=cb, op0=ALU.mult, op1=ALU.add),
                  r=[f"pre{cc}", "cols"], w=["cacc"])
            for tap in range(1, 4):
                P.dve(lambda e, cc=cc, cw=cw, tap=tap: e.scalar_tensor_tensor(
                    out=cacc, in0=pre[:, cc, tap:tap + 1024], scalar=cw[:, tap:tap + 1], in1=cacc,
                    op0=ALU.mult, op1=ALU.add), r=[f"pre{cc}", "cacc", "cols"], w=["cacc"])
            if cc < 4:
                dst, dk = xTc[:, cc, :], f"xTc{cc}"
            elif cc == 4:
                dst, dk = BTc, "BTc"
            else:
                dst, dk = CT[:, g, :], f"CT{g}"
            P.act(lambda e, dst=dst: e.activation(out=dst, in_=cacc, func=AF.Silu), r=["cacc"], w=[dk])
        if g == 0:
            dump("xTc0", xTc, [128, 4, T], [f"xTc{c}" for c in range(4)], BF16)
            dump("CT0", CT[:, 0, :], [128, T], ["CT0"], BF16)
        P.dve(lambda e: e.memset(Sst, 0.0), w=["Sst"])
        for c in range(NT):
            tk = slice(c * 128, (c + 1) * 128)
            hs = slice(g * 8, g * 8 + 8)
            pbx = psb[4][:, :].bitcast(BF16)
            for cc in range(4):
                P.pe(lambda e, cc=cc, tk=tk: e.transpose(out=pbx[:, cc * 128:(cc + 1) * 128], in_=xTc[:, cc, tk],
                                                        identity=identb[:, :]),
                     r=[f"xTc{cc}", "identb"], w=["ps4"])
            P.pe(lambda e, tk=tk: e.transpose(out=pbx[:, 512:640], in_=BTc[:, tk], identity=identb[:, :]),
                 r=["BTc", "identb"], w=["ps4"])
            P.act(lambda e: e.activation(out=xtok, in_=pbx[:, 0:512], func=AF.Copy), r=["ps4"], w=["xtok"])
            P.act(lambda e: e.activation(out=Btok, in_=pbx[:, 512:640], func=AF.Copy), r=["ps4"], w=["Btok"])
            P.dve(lambda e, c=c, hs=hs: e.tensor_tensor(out=h8(xdt), in0=h8(pbx[:, 0:512]), in1=bc(dt3[:, c, hs], 64),
                                                        op=ALU.mult), r=["ps4", "dtall"], w=["xdt"])
            P.dve(lambda e, c=c, hs=hs: e.tensor_tensor(out=h8(xdte), in0=h8(xdt), in1=bc(dte3[:, c, hs], 64),
                                                        op=ALU.mult), r=["xdt", f"dte{c}"], w=["xdte"])
            P.pe(lambda e, tk=tk: e.matmul(psb[5][:, 0:128], lhsT=BTc[:, tk], rhs=CT[:, g, tk], start=True, stop=True),
                 r=["BTc", f"CT{g}"], w=["ps5"])
            P.dve(lambda e: e.tensor_tensor(out=CBm, in0=psb[5][:, 0:128], in1=tri, op=ALU.mult),
                  r=["ps5", "consts"], w=["CBm"])
            for hq in range(2):
                pbs = 6 + hq
                for hh in range(4):
                    h = g * 8 + hq * 4 + hh
                    lb = hh % 2
                    P.act(lambda e, c=c, h=h, lb=lb: e.activation(out=Lh[lb], in_=strict, func=AF.Identity,
                                                                scale=a3[:, c, h:h + 1]),
                          r=["consts", f"a{c}"], w=[f"Lh{lb}"])
                    P.pe(lambda e, hh=hh, lb=lb, pbs=pbs: e.matmul(psb[pbs][:, hh * 128:(hh + 1) * 128], lhsT=Lh[lb],
                                                                 rhs=tri, start=True, stop=True),
                         r=[f"Lh{lb}", "consts"], w=[f"ps{pbs}"])
                P.act(lambda e, pbs=pbs: e.activation(out=dec, in_=psb[pbs][:, :], func=AF.Exp), r=[f"ps{pbs}"], w=["dec"])
                P.dve(lambda e, hq=hq: e.tensor_tensor(
                    out=Mh[:, hq * 512:(hq + 1) * 512].rearrange("p (h d) -> p h d", h=4),
                    in0=dec.rearrange("p (h d) -> p h d", h=4),
                    in1=CBm.unsqueeze(1).to_broadcast([128, 4, 128]), op=ALU.mult),
                    r=["dec", "CBm"], w=[f"Mh{hq}"])
            for hh in range(8):
                P.pe(lambda e, hh=hh: e.matmul(psb[0][:, hh * 64:(hh + 1) * 64], lhsT=Mh[:, hh * 128:(hh + 1) * 128],
                                               rhs=xdt[:, hh * 64:(hh + 1) * 64], start=True, stop=True),
                     r=[f"Mh{hh // 4}", "xdt"], w=["ps0"])
            if c > 0:
                P.pe(lambda e, tk=tk: e.matmul(psb[1][:, :], lhsT=CT[:, g, tk], rhs=Sbf, start=True, stop=True),
                     r=[f"CT{g}", "Sbf"], w=["ps1"])
                P.dve(lambda e, c=c, hs=hs: e.tensor_tensor(out=h8(ytmp), in0=h8(psb[1][:, :]),
                                                            in1=bc(eacum3[:, c, hs], 64), op=ALU.mult),
                      r=["ps1", f"eacum{c}"], w=["ytmp"])
                P.dve(lambda e: e.tensor_tensor(out=ytmp, in0=ytmp, in1=psb[0][:, :], op=ALU.add),
                      r=["ytmp", "ps0"], w=["ytmp"])
            else:
                P.dve(lambda e: e.tensor_copy(out=ytmp, in_=psb[0][:, :]), r=["ps0"], w=["ytmp"])
            P.dve(lambda e, g=g: e.tensor_tensor(out=h8(dec), in0=h8(xtok),
                                                 in1=bc(rows[:, R_DSK + g * 8:R_DSK + g * 8 + 8], 64), op=ALU.mult),
                  r=["xtok", "rows"], w=["dec"])
            yb = ybuf[c % 2]
            P.dve(lambda e, yb=yb: e.tensor_tensor(out=yb, in0=ytmp, in1=dec, op=ALU.add),
                  r=["ytmp", "dec"], w=[f"ybuf{c % 2}"])
            if debug and g == 0 and c == 0:
                dump("d_Mh", Mh, [128, 1024], ["Mh0", "Mh1"], BF16)
                dump("d_xdt", xdt, [128, 512], ["xdt"], BF16)
                dump("d_xtok", xtok, [128, 512], ["xtok"], BF16)
                dump("d_CBm", CBm, [128, 128], ["CBm"])
                dump("d_ytmp", ytmp, [128, 512], ["ytmp"])
                dump("d_dsx", dec, [128, 512], ["dec"])
                dump("d_yb", yb, [128, 512], [f"ybuf{c % 2}"])
                dump("d_a", av, [128, 256], [f"a{i}" for i in range(NT)])
                dump("d_acum", acum, [128, 256], [f"acum{i}" for i in range(NT)])
            P.dma("sp", lambda e, yb=yb, c=c, g=g: e.dma_start(out=yloc_d[c * 128:(c + 1) * 128, g * 512:(g + 1) * 512], in_=yb),
                  r=[f"ybuf{c % 2}"], w=["yloc_d"], group=f"ybuf{c % 2}")
            if debug and upto == 1.5 and g == 0 and c == 0:
                dump_dram("yloc", yloc_d, [T, D], ["yloc_d"])
                return finish(nc, P, dbg_outs)
            P.pe(lambda e: e.matmul(psb[5][:, :], lhsT=Btok, rhs=xdte, start=True, stop=True),
                 r=["Btok", "xdte"], w=["ps5"])
            P.dve(lambda e, c=c, hs=hs: e.tensor_tensor(out=h8(Sst), in0=h8(Sst), in1=bc(cdec3[:, c, hs], 64), op=ALU.mult),
                  r=["Sst", f"cdec{c}"], w=["Sst"])
            P.dve(lambda e: e.tensor_tensor(out=Sst, in0=Sst, in1=psb[5][:, :], op=ALU.add), r=["Sst", "ps5"], w=["Sst"])
            if c < NT - 1:
                P.act(lambda e: e.activation(out=Sbf, in_=Sst, func=AF.Copy), r=["Sst"], w=["Sbf"])
        P.dma("sp", lambda e, g=g: e.dma_start(out=ag2_in.ap()[:, g * 512:(g + 1) * 512], in_=Sst),
              r=["Sst"], w=["ag2_in"], group="ag2i")
    dump_dram("yloc", yloc_d, [T, D], ["yloc_d"])
    P.barrier(barscr)
    if upto <= 2:
        return finish(nc, P, dbg_outs)

    glrT = sparef[0:16, 0:1024]
    wgsb = sparef[0:16, 1024:2048]
    P.dma("sp", lambda e: e.dma_start(out=wgsb, in_=wgate_d), w=["wgsb"], group="par")
    wglr, wglr_k = load_w(win_v[:, :, OFF_GLR:OFF_GLR + 16])
    for half in range(2):
        for k in range(16):
            P.pe(lambda e, k=k, half=half: e.matmul(psb[half][0:16, :], lhsT=wglr[:, k, :],
                                                    rhs=hT[:, k, half * 512:(half + 1) * 512], start=(k == 0), stop=(k == 15)),
                 r=[wglr_k] + hkeys, w=[f"ps{half}"])
        P.act(lambda e, half=half: e.activation(out=glrT[:, half * 512:(half + 1) * 512], in_=psb[half][0:16, :], func=AF.Copy),
              r=[f"ps{half}"], w=["glrT"])
    qT1 = WK[:, 0:1024]
    kT1 = WK[:, 1024:2048]
    Sg = WK[:, 2048:3072].rearrange("p (a b) -> p a b", a=2)
    obuf = [WK[:, 3072:3584], WK[:, 3584:4096]]
    tA = WK[:, 4096:5120]
    vtok = B32[:, 0:4096].rearrange("p (a b) -> p a b", a=8)
    qrel = B32[:, 4096:6144].rearrange("p (a b) -> p a b", a=2)
    krel = B32[:, 6144:8192].rearrange("p (a b) -> p a b", a=2)
    qint = B32[:, 8192:10240].rearrange("p (a b) -> p a b", a=2)
    kendT = B32[:, 10240:12288].rearrange("p (a b) -> p a b", a=2)
    ktok = B32[:, 12288:14336].rearrange("p (a b) -> p a b", a=8)
    Sgb = B32[:, 14336:15360].rearrange("p (a b) -> p a b", a=2)
    attm = B32[:, 15360:15488]
    r3f = RING[3][:, :, :].rearrange("p a b -> p (a b)").bitcast(F32)
    cs = r3f[:, 0:1024]
    Gs = r3f[:, 1024:2048]
    tB = r3f[:, 2048:3072]
    tC = r3f[:, 3072:4096]
    gla_ring = [0, 1, 2]
    grc = [0]

    def load_w3(src_ap):
        s = gla_ring[grc[0] % 3]
        grc[0] += 1
        a, bb = src_ap.shape[1], src_ap.shape[2]
        view = RING[s][:, :, :].rearrange("p a b -> p (a b)")[:, 0:a * bb].rearrange("p (a b) -> p a b", a=a)
        P.dma("pool", lambda e: e.dma_start(out=view, in_=src_ap), w=[f"ring{s}"], group=f"ring{s}")
        return view, f"ring{s}"

    ones_bc = onesf[:, 0:1].to_broadcast([128, 1024])
    for hd in range(4):
        wq, wq_k = load_w3(win_v[:, :, OFF_Q + hd * 256:OFF_Q + (hd + 1) * 256])
        wk2, wk2_k = load_w3(win_v[:, :, OFF_K + hd * 256:OFF_K + (hd + 1) * 256])
        wv2, wv2_k = load_w3(win_v[:, :, OFF_V + hd * 512:OFF_V + (hd + 1) * 512])
        for i in range(NT):
            pb = 4 + i % 2
            for k in range(16):
                P.pe(lambda e, i=i, k=k, pb=pb: e.matmul(psb[pb][:, :], lhsT=hT[:, k, i * 128:(i + 1) * 128],
                                                         rhs=wv2[:, k, :], start=(k == 0), stop=(k == 15)),
                     r=[wv2_k, hkeys[i]], w=[f"ps{pb}"])
            P.dve(lambda e, i=i, pb=pb: e.tensor_copy(out=vtok[:, i, :], in_=psb[pb][:, :]), r=[f"ps{pb}"], w=["vtok"])
        for dc in range(2):
            gcx = hd * 2 + dc
            for (wsrc, wkey, dst1, dkey, pb0) in ((wq, wq_k, qT1, "qT", 0), (wk2, wk2_k, kT1, "kT", 2)):
                for half in range(2):
                    pb = pb0 + half
                    for k in range(16):
                        P.pe(lambda e, wsrc=wsrc, dc=dc, k=k, half=half, pb=pb: e.matmul(
                            psb[pb][:, :], lhsT=wsrc[:, k, dc * 128:(dc + 1) * 128],
                            rhs=hT[:, k, half * 512:(half + 1) * 512], start=(k == 0), stop=(k == 15)),
                            r=[wkey] + hkeys, w=[f"ps{pb}"])
                    P.act(lambda e, dst1=dst1, half=half, pb=pb: e.activation(
                        out=dst1[:, half * 512:(half + 1) * 512], in_=psb[pb][:, :], func=AF.Copy),
                        r=[f"ps{pb}"], w=[dkey])
            for half in range(2):
                pb = 6 + half
                P.pe(lambda e, gcx=gcx, half=half, pb=pb: e.matmul(
                    psb[pb][:, :], lhsT=wgsb[:, gcx * 128:(gcx + 1) * 128], rhs=glrT[:, half * 512:(half + 1) * 512],
                    start=True, stop=True), r=["wgsb", "glrT"], w=[f"ps{pb}"])
                P.act(lambda e, gcx=gcx, half=half, pb=pb: e.activation(
                    out=tA[:, half * 512:(half + 1) * 512], in_=psb[pb][:, :], func=AF.Exp, scale=-1.0,
                    bias=negb[:, gcx:gcx + 1]), r=[f"ps{pb}", "negb"], w=["tA"])
            P.act(lambda e: e.activation(out=tA, in_=tA, func=AF.Ln, bias=1.0), r=["tA"], w=["tA"])
            if debug and hd == 0 and dc == 0:
                dump("sp0", tA, [128, 1024], ["tA"])
            P.dve(lambda e: e.tensor_tensor_scan(out=cs, data0=resetm, data1=tA, initial=0.0, op0=ALU.mult, op1=ALU.add),
                  r=["tA", "consts"], w=["cs", "ring3"])
            P.dve(lambda e: e.tensor_tensor_scan(out=Gs, data0=ones_bc, data1=tA, initial=0.0, op0=ALU.mult, op1=ALU.add),
                  r=["tA", "onesf"], w=["Gs", "ring3"])
            cs3 = cs.rearrange("p (c l) -> p c l", c=16)
            P.dve(lambda e: e.tensor_copy(out=midv, in_=cs3[:, :, 32]), r=["cs"], w=["midv"])
            P.dve(lambda e: e.tensor_copy(out=lastv, in_=cs3[:, :, 63]), r=["cs"], w=["lastv"])
            P.act(lambda e, dc=dc: e.activation(out=dcy[:, dc * 16:(dc + 1) * 16], in_=lastv, func=AF.Exp, scale=-1.0 / 16),
                  r=["lastv"], w=[f"dcy{dc}"])
            P.dve(lambda e: e.tensor_tensor(out=tB.rearrange("p (c l) -> p c l", c=16), in0=cs3, in1=bc(midv, 64),
                                            op=ALU.subtract), r=["cs", "midv"], w=["tB"])
            P.act(lambda e: e.activation(out=tC, in_=tB, func=AF.Exp, scale=-1.0 / 16), r=["tB"], w=["tC"])
            P.dve(lambda e, dc=dc: e.scalar_tensor_tensor(out=qrel[:, dc, :], in0=qT1, scalar=1.0 / 16, in1=tC,
                                                          op0=ALU.mult, op1=ALU.mult), r=["qT", "tC"], w=["qrel"])
            P.act(lambda e: e.activation(out=tC, in_=tB, func=AF.Exp, scale=1.0 / 16), r=["tB"], w=["tC"])
            P.dve(lambda e, dc=dc: e.tensor_tensor(out=krel[:, dc, :], in0=kT1, in1=tC, op=ALU.mult),
                  r=["kT", "tC"], w=["krel"])
            P.act(lambda e: e.activation(out=tC, in_=cs, func=AF.Exp, scale=-1.0 / 16), r=["cs"], w=["tC"])
            P.dve(lambda e, dc=dc: e.scalar_tensor_tensor(out=qint[:, dc, :], in0=qT1, scalar=1.0 / 16, in1=tC,
                                                          op0=ALU.mult, op1=ALU.mult), r=["qT", "tC"], w=["qint"])
            P.dve(lambda e: e.tensor_tensor(out=tB.rearrange("p (c l) -> p c l", c=16), in0=cs3, in1=bc(lastv, 64),
                                            op=ALU.subtract), r=["cs", "lastv"], w=["tB"])
            P.act(lambda e: e.activation(out=tC, in_=tB, func=AF.Exp, scale=1.0 / 16), r=["tB"], w=["tC"])
            P.dve(lambda e, dc=dc: e.tensor_tensor(out=kendT[:, dc, :], in0=kT1, in1=tC, op=ALU.mult),
                  r=["kT", "tC"], w=["kendT"])
            P.act(lambda e: e.activation(out=tC, in_=Gs, func=AF.Exp, scale=-1.0 / 16), r=["Gs"], w=["tC"])
            P.dve(lambda e, dc=dc, gcx=gcx: e.scalar_tensor_tensor(out=QG[:, gcx, :], in0=qT1, scalar=1.0 / 16,
                                                                   in1=tC, op0=ALU.mult, op1=ALU.mult),
                  r=["qT", "tC"], w=[f"QG{gcx}"])
            P.dve(lambda e, gcx=gcx: e.tensor_copy(out=rt[:, 32 + gcx:33 + gcx], in_=tC[:, 1023:1024]), r=["tC"], w=["dg"])
        if debug and hd == 0:
            dump("qrel0", qrel, [128, 2, T], ["qrel"], BF16)
            dump("krel0", krel, [128, 2, T], ["krel"], BF16)
        for i in range(NT):
            pbx = psb[4 + i % 2][:, :].bitcast(BF16)
            for dc in range(2):
                P.pe(lambda e, i=i, dc=dc, pbx=pbx: e.transpose(out=pbx[:, dc * 128:(dc + 1) * 128],
                                                               in_=kendT[:, dc, i * 128:(i + 1) * 128], identity=identb[:, :]),
                     r=["kendT", "identb"], w=[f"ps{4 + i % 2}"])
            P.act(lambda e, i=i, pbx=pbx: e.activation(out=ktok[:, i, :], in_=pbx[:, 0:256], func=AF.Copy),
                  r=[f"ps{4 + i % 2}"], w=["ktok"])
        P.dve(lambda e: e.memset(Sg.rearrange("p a b -> p (a b)"), 0.0), w=["Sg"])
        P.dve(lambda e: e.memset(Sgb.rearrange("p a b -> p (a b)"), 0.0), w=["Sgb"])
        for i in range(NT):
            tk = slice(i * 128, (i + 1) * 128)
            po = i % 2
            for dc in range(2):
                P.pe(lambda e, dc=dc, tk=tk: e.matmul(psb[2][:, 0:128], lhsT=krel[:, dc, tk], rhs=qrel[:, dc, tk],
                                                      start=(dc == 0), stop=(dc == 1)),
                     r=["krel", "qrel"], w=["ps2"])
            P.dve(lambda e: e.tensor_tensor(out=attm, in0=psb[2][:, 0:128], in1=glamask, op=ALU.mult),
                  r=["ps2", "consts"], w=["attm"])
            for hf in range(2):
                cidx = 2 * i + hf
                rs = slice(hf * 64, (hf + 1) * 64)
                tk64 = slice(i * 128 + hf * 64, i * 128 + hf * 64 + 64)
                for dc in range(2):
                    P.pe(lambda e, dc=dc, tk64=tk64, rs=rs, po=po: e.matmul(
                        psb[po][rs, :], lhsT=qint[:, dc, tk64], rhs=Sgb[:, dc, :], start=(dc == 0), stop=False),
                        r=["qint", "Sgb"], w=[f"ps{po}"])
                for dc in range(2):
                    pu = 6 + dc
                    P.pe(lambda e, dc=dc, rs=rs, i=i, pu=pu: e.matmul(
                        psb[pu][:, :], lhsT=ktok[rs, i, dc * 128:(dc + 1) * 128], rhs=vtok[rs, i, :], start=True, stop=True),
                        r=["ktok", "vtok"], w=[f"ps{pu}"])
                    P.dve(lambda e, dc=dc, cidx=cidx, pu=pu: e.scalar_tensor_tensor(
                        out=Sg[:, dc, :], in0=Sg[:, dc, :], scalar=dcy[:, dc * 16 + cidx:dc * 16 + cidx + 1],
                        in1=psb[pu][:, :], op0=ALU.mult, op1=ALU.add), r=["Sg", f"dcy{dc}", f"ps{pu}"], w=["Sg"])
                    P.act(lambda e, dc=dc: e.activation(out=Sgb[:, dc, :], in_=Sg[:, dc, :], func=AF.Copy), r=["Sg"], w=["Sgb"])
            P.pe(lambda e, i=i, po=po: e.matmul(psb[po][:, :], lhsT=attm, rhs=vtok[:, i, :], start=False, stop=True),
                 r=["attm", "vtok"], w=[f"ps{po}"])
            ob = obuf[i % 2]
            P.act(lambda e, ob=ob, po=po: e.activation(out=ob, in_=psb[po][:, :], func=AF.Copy), r=[f"ps{po}"], w=[f"obuf{i % 2}"])
            P.dma("sp", lambda e, ob=ob, i=i, hd=hd: e.dma_start(out=oloc_d[i * 128:(i + 1) * 128, hd * 512:(hd + 1) * 512], in_=ob),
                  r=[f"obuf{i % 2}"], w=["oloc_d"], group=f"obuf{i % 2}")
        for dc in range(2):
            gcx = hd * 2 + dc
            P.dma("sp", lambda e, dc=dc, gcx=gcx: e.dma_start(out=ag2_in.ap()[:, 2048 + gcx * 512:2048 + (gcx + 1) * 512],
                                                            in_=Sg[:, dc, :]), r=["Sg"], w=["ag2_in"], group="ag2i")
    P.dma("sp", lambda e: e.dma_start(out=ag2_in.ap()[:, 6176:6184], in_=rt[:, 32:40]), r=["dg"], w=["ag2_in"], group="ag2i")
    dump_dram("oloc", oloc_d, [T, D], ["oloc_d"])
    P.barrier(barscr)
    if upto <= 3:
        return finish(nc, P, dbg_outs)

    P.coll(lambda e: e.collective_compute("AllGather", ALU.bypass, replica_groups=[list(range(NCORES))],
                                          ins=[ag2_in.ap()], outs=[ag2_out.ap()]),
           r=["ag2_in"], w=["ag2_out"], group="cc2")
    if upto >= 7:
        for k in range(2, n_groups):
            weight_ag(k)
    ag2_v = ag2_out.ap().rearrange("(r p) f -> r p f", p=128)
    SinG = RING[0][:, :, :].rearrange("p a b -> p (a b)").bitcast(F32)
    SinS = RING[1][:, :, :].rearrange("p a b -> p (a b)").bitcast(F32)[:, 0:2048]
    slab = [WK[:, 0:XW], B32f[:, 0:XW]]
    djp = rt[:, 0:40]
    P.dve(lambda e: e.memset(SinG, 0.0), w=["ring0"])
    P.dve(lambda e: e.memset(SinS, 0.0), w=["ring1"])
    for j in range(NCORES - 1):
        sl = slab[j % 2]
        P.dma("sp", lambda e, j=j, sl=sl: e.dma_start(out=sl, in_=ag2_v[j]), r=["ag2_out"], w=[f"slab{j % 2}"],
              group=f"slab{j % 2}")
        fj = flg[:, 1 + j:2 + j]
        P.dve(lambda e, sl=sl, fj=fj, j=j: e.tensor_scalar(out=djp, in0=sl[:, 6144:6184], scalar1=fj, scalar2=omf[:, j:j + 1],
                                                         op0=ALU.mult, op1=ALU.add), r=[f"slab{j % 2}", "cols", "omf"], w=["djp"])
        P.dve(lambda e: e.tensor_tensor(out=SinS.rearrange("p (h d) -> p h d", h=32), in0=SinS.rearrange("p (h d) -> p h d", h=32),
                                        in1=bc(djp[:, 0:32], 64), op=ALU.mult), r=["djp", "ring1"], w=["ring1"])
        P.dve(lambda e, sl=sl, fj=fj: e.scalar_tensor_tensor(out=SinS, in0=sl[:, 0:2048], scalar=fj, in1=SinS,
                                                             op0=ALU.mult, op1=ALU.add), r=[f"slab{j % 2}", "ring1", "cols"], w=["ring1"])
        P.dve(lambda e: e.tensor_tensor(out=SinG.rearrange("p (h d) -> p h d", h=8), in0=SinG.rearrange("p (h d) -> p h d", h=8),
                                        in1=bc(djp[:, 32:40], 512), op=ALU.mult), r=["djp", "ring0"], w=["ring0"])
        P.dve(lambda e, sl=sl, fj=fj: e.scalar_tensor_tensor(out=SinG, in0=sl[:, 2048:6144], scalar=fj, in1=SinG,
                                                             op0=ALU.mult, op1=ALU.add), r=[f"slab{j % 2}", "ring0", "cols"], w=["ring0"])
    SinGb = E32[:, 12288:16384].rearrange("p (a b) -> p a b", a=8)
    SinSb = WK[:, 5576:6600].bitcast(BF16)
    P.act(lambda e: e.activation(out=SinGb.rearrange("p a b -> p (a b)"), in_=SinG, func=AF.Copy),
          r=["ring0", "glrT", "wgsb"], w=["SinGb", "glrT", "wgsb"])
    P.act(lambda e: e.activation(out=SinSb, in_=SinS, func=AF.Copy), r=["ring1"], w=["SinSb"])
    dump("SinS", SinS, [128, 2048], ["ring1"])
    dump("SinG", SinG, [128, 4096], ["ring0"])
    P.barrier(barscr)
    if upto <= 3.5:
        return finish(nc, P, dbg_outs)

    mixed = B32[:, :].rearrange("p (a b) -> p a b", a=NT)
    sgrow = WK[:, 0:512]
    ylb = [WK[:, 512:1024], WK[:, 1024:1536]]
    olb = [WK[:, 1536:2048], WK[:, 2048:2560]]
    t1 = WK[:, 2560:3072]; t2 = WK[:, 3072:3584]; t3 = WK[:, 3584:4096]; t4 = WK[:, 4096:4608]; t5 = WK[:, 4608:5120]
    ggrow = rows[:, R_GG:R_GG + 512]
    for j in range(4):
        P.dma("sp", lambda e, j=j: e.dma_start(out=sgrow, in_=rows2_d[:, 2048 + j * 512:2048 + (j + 1) * 512]),
              w=["sgrow"], group="sgrow")
        wz, wz_k = load_w(win_v[:, :, OFF_Z + j * 512:OFF_Z + (j + 1) * 512])
        wr2, wr2_k = load_w(win_v[:, :, OFF_R + j * 512:OFF_R + (j + 1) * 512])
        wma, wma_k = load_w(win_v[:, :, OFF_MA + j * 512:OFF_MA + (j + 1) * 512])
        wmb, wmb_k = load_w(win_v[:, :, OFF_MB + j * 512:OFF_MB + (j + 1) * 512])
        for i in range(NT):
            tk = slice(i * 128, (i + 1) * 128)
            bsel = i % 2
            P.dma("sp", lambda e, i=i, j=j, bsel=bsel: e.dma_start(out=ylb[bsel], in_=yloc_d[i * 128:(i + 1) * 128, j * 512:(j + 1) * 512]),
                  r=["yloc_d"], w=[f"ylb{bsel}"], group=f"ylb{bsel}")
            P.dma("sp", lambda e, i=i, j=j, bsel=bsel: e.dma_start(out=olb[bsel], in_=oloc_d[i * 128:(i + 1) * 128, j * 512:(j + 1) * 512]),
                  r=["oloc_d"], w=[f"olb{bsel}"], group=f"olb{bsel}")
            for bi, (wsrc, wkey) in enumerate(((wz, wz_k), (wr2, wr2_k), (wma, wma_k), (wmb, wmb_k))):
                for k in range(16):
                    P.pe(lambda e, bi=bi, wsrc=wsrc, k=k, tk=tk: e.matmul(psb[bi][:, :], lhsT=hT[:, k, tk], rhs=wsrc[:, k, :],
                                                                        start=(k == 0), stop=(k == 15)),
                         r=[wkey, hkeys[i]], w=[f"ps{bi}"])
            P.pe(lambda e, j=j, tk=tk: e.matmul(psb[4][:, :], lhsT=CT[:, j, tk], rhs=SinSb[:, j * 512:(j + 1) * 512],
                                                start=True, stop=True), r=[f"CT{j}", "SinSb"], w=["ps4"])
            for dc in range(2):
                P.pe(lambda e, j=j, dc=dc, tk=tk: e.matmul(psb[5][:, :], lhsT=QG[:, j * 2 + dc, tk], rhs=SinGb[:, j * 2 + dc, :],
                                                           start=(dc == 0), stop=(dc == 1)),
                     r=[f"QG{j * 2 + dc}", "SinGb"], w=["ps5"])
            yl, ol = ylb[bsel], olb[bsel]
            hs = slice(j * 8, j * 8 + 8)
            P.act(lambda e: e.activation(out=t1, in_=psb[0][:, :], func=AF.Silu), r=["ps0"], w=["t1"])
            P.dve(lambda e, i=i, hs=hs: e.tensor_tensor(out=h8(t2), in0=h8(psb[4][:, :]), in1=bc(eglob3[:, i, hs], 64), op=ALU.mult),
                  r=["ps4", f"eglob{i}"], w=["t2"])
            P.dve(lambda e, yl=yl: e.tensor_tensor(out=t2, in0=t2, in1=yl, op=ALU.add), r=["t2", f"ylb{bsel}"], w=["t2"])
            P.dve(lambda e: e.tensor_tensor(out=t2, in0=t2, in1=t1, op=ALU.mult), r=["t2", "t1"], w=["t2"])
            P.act(lambda e: e.activation(out=t5, in_=t2, func=AF.Square, accum_out=ss1[:, 0:1]), r=["t2"], w=["t5", "ss1"])
            P.act(lambda e: e.activation(out=ss1[:, 0:1], in_=ss1[:, 0:1], func=AF.Sqrt, scale=1.0 / 512, bias=epsc), r=["ss1", "epsc"], w=["ss1"])
            P.dve(lambda e: e.reciprocal(out=ss1[:, 0:1], in_=ss1[:, 0:1]), r=["ss1"], w=["ss1"])
            P.dve(lambda e: e.scalar_tensor_tensor(out=t2, in0=t2, scalar=ss1[:, 0:1], in1=sgrow, op0=ALU.mult, op1=ALU.mult),
                  r=["t2", "ss1", "sgrow"], w=["t2"])
            P.dve(lambda e, ol=ol: e.tensor_tensor(out=t3, in0=psb[5][:, :], in1=ol, op=ALU.add), r=["ps5", f"olb{bsel}"], w=["t3"])
            P.act(lambda e: e.activation(out=t5, in_=t3, func=AF.Square, accum_out=ss2[:, 0:1]), r=["t3"], w=["t5", "ss2"])
            P.act(lambda e: e.activation(out=ss2[:, 0:1], in_=ss2[:, 0:1], func=AF.Sqrt, scale=1.0 / 512, bias=epsc), r=["ss2", "epsc"], w=["ss2"])
            P.dve(lambda e: e.reciprocal(out=ss2[:, 0:1], in_=ss2[:, 0:1]), r=["ss2"], w=["ss2"])
            P.act(lambda e: e.activation(out=t4, in_=psb[1][:, :], func=AF.Silu), r=["ps1"], w=["t4"])
            P.dve(lambda e: e.scalar_tensor_tensor(out=t3, in0=t3, scalar=ss2[:, 0:1], in1=ggrow, op0=ALU.mult, op1=ALU.mult),
                  r=["t3", "ss2", "rows"], w=["t3"])
            P.dve(lambda e: e.tensor_tensor(out=t3, in0=t3, in1=t4, op=ALU.mult), r=["t3", "t4"], w=["t3"])
            P.act(lambda e: e.activation(out=t1, in_=psb[2][:, :], func=AF.Sigmoid), r=["ps2"], w=["t1"])
            P.act(lambda e: e.activation(out=t4, in_=psb[3][:, :], func=AF.Sigmoid), r=["ps3"], w=["t4"])
            P.dve(lambda e: e.tensor_tensor(out=t1, in0=t1, in1=t2, op=ALU.mult), r=["t1", "t2"], w=["t1"])
            P.dve(lambda e: e.tensor_tensor(out=t4, in0=t4, in1=t3, op=ALU.mult), r=["t4", "t3"], w=["t4"])
            P.dve(lambda e, i=i, j=j: e.tensor_tensor(out=mixed[:, i, j * 512:(j + 1) * 512], in0=t1, in1=t4, op=ALU.add),
                  r=["t1", "t4"], w=[("mix", i)])
    mixkeys = [("mix", i) for i in range(NT)]
    dump("mixed", mixed, [128, NT, D], mixkeys, BF16)
    P.barrier(barscr)
    if upto <= 4:
        return finish(nc, P, dbg_outs)

    mixedT = A32
    for i in range(NT):
        for q4 in range(4):
            pbi = 4 + (i * 4 + q4) % 4
            pbx = psb[pbi][:, :].bitcast(BF16)
            for kk in range(4):
                k = q4 * 4 + kk
                P.pe(lambda e, i=i, k=k, kk=kk, pbx=pbx: e.transpose(out=pbx[:, kk * 128:(kk + 1) * 128],
                                                                    in_=mixed[:, i, k * 128:(k + 1) * 128], identity=identb[:, :]),
                     r=[("mix", i), "identb"], w=[f"ps{pbi}"])
            src = pbx[:, 0:512].rearrange("p (a b) -> p a b", a=4)
            dstv = mixedT[:, q4 * 4:q4 * 4 + 4, i * 128:(i + 1) * 128]
            if q4 % 2 == 0:
                P.act(lambda e, src=src, dstv=dstv: e.activation(out=dstv, in_=src, func=AF.Copy), r=[f"ps{pbi}"], w=[("mT", i)])
            else:
                P.dve(lambda e, src=src, dstv=dstv: e.tensor_copy(out=dstv, in_=src), r=[f"ps{pbi}"], w=[("mT", i)])
    P.barrier(barscr)
    wo = [load_w(wout_d.rearrange("(k p) c -> p k c", p=128)[:, :, nb * 512:(nb + 1) * 512]) for nb in range(4)]
    g1row = WK[:, 0:2048]
    xtl = WK[:, 2048:4096]
    x1b = WK[:, 4096:6144]
    P.dma("sp", lambda e: e.dma_start(out=g1row, in_=ag1_flat[4096:6144].partition_broadcast(128)), r=["ag1_out"], w=["g1row"], group="par")
    xs5 = [B32f[:, 0:2048], B32f[:, 2048:4096]]
    h2f = B32f[:, 6144:8192].rearrange("p (a b) -> p a b", a=16)
    junk5 = B32f[:, 6144:8192]
    wr3 = B32f[:, 4096:5120].rearrange("p (a b) -> p a b", a=16)
    P.dma("sp", lambda e: e.dma_start(out=B32f[:, 4096:5120], in_=wr_d), w=["wr3"], group="par")
    rch = WK[:, 6144:6600]
    s_ = rch[:, 0:64]; ch = rch[:, 64:128]; ch2 = rch[:, 128:192]; eq = rch[:, 192:256]; chm = rch[:, 256:320]; sel = rch[:, 320:384]
    m1 = rch[:, 384:392]; m2 = rch[:, 392:400]; gs = rch[:, 400:408]; mx8 = rch[:, 408:416]; gmask = rch[:, 416:424]
    gm1 = rch[:, 424:432]; den = rch[:, 432:433]
    for i in range(NT):
        tk = slice(i * 128, (i + 1) * 128)
        P.dma("sp", lambda e, i=i: e.dma_start(out=xtl, in_=x_d[i * 128:(i + 1) * 128, :]), w=["xtl"], group="xtl")
        for nb in range(4):
            for k in range(16):
                P.pe(lambda e, nb=nb, k=k, tk=tk: e.matmul(psb[nb][:, :], lhsT=mixedT[:, k, tk], rhs=wo[nb][0][:, k, :],
                                                         start=(k == 0), stop=(k == 15)),
                     r=[("mT", i), wo[nb][1]], w=[f"ps{nb}"])
            P.dve(lambda e, nb=nb: e.tensor_tensor(out=x1b[:, nb * 512:(nb + 1) * 512], in0=psb[nb][:, :], in1=g1row[:, nb * 512:(nb + 1) * 512], op=ALU.mult),
                  r=[f"ps{nb}", "g1row"], w=["x1b"])
            P.dve(lambda e, nb=nb: e.tensor_tensor(out=x1b[:, nb * 512:(nb + 1) * 512], in0=x1b[:, nb * 512:(nb + 1) * 512], in1=xtl[:, nb * 512:(nb + 1) * 512], op=ALU.add),
                  r=["x1b", "xtl"], w=["x1b"])
        P.dma("sp", lambda e, i=i: e.dma_start(out=x1_spill[i * 128:(i + 1) * 128, :], in_=x1b), r=["x1b"], w=["x1_spill"], group="x1b")
        if upto == 4.6:
            continue
        norm_tile(i, x1b, 128, A2, 48, h2T, slice(i * 128, (i + 1) * 128), "x1b", xs5, junk5, ("h2T", i), f32dst=h2f, jkey="h2f")
        for k in range(16):
            P.pe(lambda e, k=k: e.matmul(psb[0][:, 0:64], lhsT=h2f[:, k, :], rhs=wr3[:, k, :], start=(k == 0), stop=(k == 15)),
                 r=["h2f", "wr3"], w=["ps0"])
        P.act(lambda e: e.activation(out=s_, in_=psb[0][:, 0:64], func=AF.Sigmoid), r=["ps0"], w=["s_"])
        P.dve(lambda e: e.tensor_tensor(out=ch, in0=s_, in1=rows[:, R_RB:R_RB + 64], op=ALU.add), r=["s_", "rows"], w=["ch"])
        if upto == 4.8:
            P.dve(lambda e, i=i: e.tensor_copy(out=Gt3[:, i, :], in_=ch), r=["ch"], w=[f"G{i}"])
            continue
        ch3 = ch.rearrange("p (g e) -> p g e", g=8)
        P.dve(lambda e: e.tensor_reduce(out=m1, in_=ch3, axis=AX.X, op=ALU.max), r=["ch"], w=["m1"])
        P.dve(lambda e: e.tensor_tensor(out=eq.rearrange("p (g e) -> p g e", g=8), in0=ch3, in1=bc(m1, 8), op=ALU.is_equal),
              r=["ch", "m1"], w=["eq"])
        P.dve(lambda e: e.scalar_tensor_tensor(out=ch2, in0=eq, scalar=-8.0, in1=ch, op0=ALU.mult, op1=ALU.add), r=["eq", "ch"], w=["ch2"])
        P.dve(lambda e: e.tensor_reduce(out=m2, in_=ch2.rearrange("p (g e) -> p g e", g=8), axis=AX.X, op=ALU.max), r=["ch2"], w=["m2"])
        P.dve(lambda e: e.tensor_tensor(out=gs, in0=m1, in1=m2, op=ALU.add), r=["m1", "m2"], w=["gs"])
        P.dve(lambda e: e.max(out=mx8, in_=gs), r=["gs"], w=["mx8"])
        P.dve(lambda e: e.tensor_scalar(out=gmask, in0=gs, scalar1=mx8[:, 3:4], scalar2=None, op0=ALU.is_ge), r=["gs", "mx8"], w=["gmask"])
        P.dve(lambda e: e.tensor_scalar(out=gm1, in0=gmask, scalar1=-1.0, scalar2=4.0, op0=ALU.add, op1=ALU.mult), r=["gmask"], w=["gm1"])
        P.dve(lambda e: e.tensor_tensor(out=chm.rearrange("p (g e) -> p g e", g=8), in0=ch3, in1=bc(gmask, 8), op=ALU.mult),
              r=["ch", "gmask"], w=["chm"])
        P.dve(lambda e: e.tensor_tensor(out=chm.rearrange("p (g e) -> p g e", g=8), in0=chm.rearrange("p (g e) -> p g e", g=8),
                                        in1=bc(gm1, 8), op=ALU.add), r=["chm", "gm1"], w=["chm"])
        P.dve(lambda e: e.max(out=mx8, in_=chm), r=["chm"], w=["mx8"])
        P.dve(lambda e: e.tensor_scalar(out=sel, in0=chm, scalar1=mx8[:, 7:8], scalar2=None, op0=ALU.is_ge), r=["chm", "mx8"], w=["sel"])
        P.dve(lambda e: e.tensor_tensor(out=sel, in0=sel, in1=s_, op=ALU.mult), r=["sel", "s_"], w=["sel"])
        P.dve(lambda e: e.tensor_reduce(out=den, in_=sel, axis=AX.X, op=ALU.add), r=["sel"], w=["den"])
        P.dve(lambda e: e.tensor_scalar(out=den, in0=den, scalar1=1e-20, scalar2=None, op0=ALU.add), r=["den"], w=["den"])
        P.dve(lambda e: e.reciprocal(out=den, in_=den), r=["den"], w=["den"])
        P.dve(lambda e, i=i: e.tensor_scalar(out=Gt3[:, i, :], in0=sel, scalar1=den, scalar2=2.5, op0=ALU.mult, op1=ALU.mult),
              r=["sel", "den"], w=[f"G{i}"])
    dump_dram("x1", x1_spill, [T, D], ["x1_spill"])
    if upto == 4.6:
        return finish(nc, P, dbg_outs)
    dump("G", Gt, [128, NT * 64], [f"G{i}" for i in range(NT)])
    dump("h2T", h2T, [128, 16, T], [("h2T", i) for i in range(NT)], BF16)
    if upto <= 5:
        return finish(nc, P, dbg_outs)
    P.barrier(barscr)

    accA = B32f.rearrange("p (a b) -> p a b", a=4)
    accB = A32f.rearrange("p (a b) -> p a b", a=4)

    def acc_t(i):
        return accA[:, i, :] if i < 4 else accB[:, i - 4, :]

    P.dve(lambda e: e.memset(B32f, 0.0), w=[("acc", i) for i in range(4)])
    P.pool(lambda e: e.memset(A32f, 0.0), w=[("acc", i) for i in range(4, 8)])
    hid = [WK[:, 0:2048].bitcast(BF16).rearrange("p (a b) -> p a b", a=4), WK[:, 2048:4096].bitcast(BF16).rearrange("p (a b) -> p a b", a=4)]
    sg_ = [WK[:, 4096:4608], WK[:, 4608:5120]]
    h2keys = [("h2T", i) for i in range(NT)]
    order = [(NEXP, wsh_d, 0, ())] if upto != 7.8 else []
    for k in range(n_groups):
        for c2 in range(NCORES):
            order.append((8 * c2 + k, gath[k].ap(), c2 * 6144, [("gath", k)]))
    for ei, (ex, wsrc_d, base, rk) in enumerate(order):
        wg, wg_k = load_w(wsrc_d[base:base + 2048, :].rearrange("(k p) c -> p k c", p=128), rk)
        wu, wu_k = load_w(wsrc_d[base + 2048:base + 4096, :].rearrange("(k p) c -> p k c", p=128), rk)
        wd, wd_k = load_w(wsrc_d[base + 4096:base + 6144, :].rearrange("(a b) c -> a (b c)", b=4)
                          .rearrange("(k p) c -> p k c", p=128), rk)
        hb = hid[ei % 2]
        hk = f"hid{ei % 2}"
        cnt = 0
        for half in range(2):
            for hc in range(4):
                pg, pu = (cnt % 2) * 2, (cnt % 2) * 2 + 1
                cnt += 1
                for (wsrc, wkey, pbk) in ((wg, wg_k, pg), (wu, wu_k, pu)):
                    for k in range(16):
                        P.pe(lambda e, wsrc=wsrc, hc=hc, k=k, half=half, pbk=pbk: e.matmul(
                            psb[pbk][:, :], lhsT=wsrc[:, k, hc * 128:(hc + 1) * 128], rhs=h2T[:, k, half * 512:(half + 1) * 512],
                            start=(k == 0), stop=(k == 15)), r=[wkey] + h2keys[half * 4:(half + 1) * 4], w=[f"ps{pbk}"])
                sgb = sg_[cnt % 2]
                P.act(lambda e, sgb=sgb, pg=pg: e.activation(out=sgb, in_=psb[pg][:, :], func=AF.Silu), r=[f"ps{pg}"], w=[f"sg{cnt % 2}"])
                P.dve(lambda e, sgb=sgb, pu=pu, hb=hb, hc=hc, half=half: e.tensor_tensor(
                    out=hb[:, hc, half * 512:(half + 1) * 512], in0=sgb, in1=psb[pu][:, :], op=ALU.mult),
                    r=[f"sg{cnt % 2}", f"ps{pu}"], w=[hk])
        cnt = 0
        for i in range(NT):
            for nb in range(4):
                pd = 4 + cnt % 4
                cnt += 1
                for hc in range(4):
                    P.pe(lambda e, hb=hb, hc=hc, i=i, nb=nb, pd=pd: e.matmul(
                        psb[pd][:, :], lhsT=hb[:, hc, i * 128:(i + 1) * 128], rhs=wd[:, hc, nb * 512:(nb + 1) * 512],
                        start=(hc == 0), stop=(hc == 3)), r=[hk, wd_k], w=[f"ps{pd}"])
                av_ = acc_t(i)[:, nb * 512:(nb + 1) * 512]
                if ex == NEXP:
                    P.dve(lambda e, av_=av_, pd=pd: e.tensor_tensor(out=av_, in0=av_, in1=psb[pd][:, :], op=ALU.add),
                          r=[f"ps{pd}", ("acc", i)], w=[("acc", i)])
                else:
                    P.dve(lambda e, av_=av_, pd=pd, i=i, ex=ex: e.scalar_tensor_tensor(
                        out=av_, in0=psb[pd][:, :], scalar=Gt3[:, i, ex:ex + 1], in1=av_, op0=ALU.mult, op1=ALU.add),
                        r=[f"ps{pd}", ("acc", i), f"G{i}"], w=[("acc", i)])
    if debug:
        dump("accA", B32f, [128, 8192], [("acc", i) for i in range(4)])
    if upto == 7.5:
        return finish(nc, P, dbg_outs)

    g2row = WK[:, 0:2048]
    nfrow = WK[:, 2048:4096]
    x1l = WK[:, 4096:6144]
    P.dma("sp", lambda e: e.dma_start(out=g2row, in_=ag1_flat[10240:12288].partition_broadcast(128)), r=["ag1_out"], w=["g2row", "hid0"], group="par")
    P.dma("sp", lambda e: e.dma_start(out=nfrow, in_=rows2_d[:, 0:2048]), w=["nfrow", "hid1"], group="par")
    for i in range(NT):
        P.dma("sp", lambda e, i=i: e.dma_start(out=x1l, in_=x1_spill[i * 128:(i + 1) * 128, :]), r=["x1_spill"], w=["x1l", "sg0", "sg1"], group="x1l")
        av_ = acc_t(i)
        P.dve(lambda e, av_=av_: e.tensor_tensor(out=av_, in0=av_, in1=g2row, op=ALU.mult), r=[("acc", i), "g2row"], w=[("acc", i)])
        P.dve(lambda e, av_=av_: e.tensor_tensor(out=av_, in0=av_, in1=x1l, op=ALU.add), r=[("acc", i), "x1l"], w=[("acc", i)])
        P.act(lambda e, av_=av_, i=i: e.activation(out=x1l, in_=av_, func=AF.Square, accum_out=ssq[:, i:i + 1]), r=[("acc", i)], w=["x1l", f"ssq{i}"])
        P.act(lambda e, i=i: e.activation(out=rstd[:, i:i + 1], in_=ssq[:, i:i + 1], func=AF.Sqrt, scale=1.0 / D, bias=epsc),
              r=[f"ssq{i}", "epsc"], w=[f"rstd{i}"])
        P.dve(lambda e, i=i: e.reciprocal(out=rstd[:, i:i + 1], in_=rstd[:, i:i + 1]), r=[f"rstd{i}"], w=[f"rstd{i}"])
        P.dve(lambda e, av_=av_, i=i: e.scalar_tensor_tensor(out=av_, in0=av_, scalar=rstd[:, i:i + 1], in1=nfrow, op0=ALU.mult, op1=ALU.mult),
              r=[("acc", i), f"rstd{i}", "nfrow"], w=[("acc", i)])
        P.dma("sp", lambda e, av_=av_, i=i: e.dma_start(out=out_d[i * 128:(i + 1) * 128, :], in_=av_), r=[("acc", i)], w=["out_d"], group="out")
    return finish(nc, P, dbg_outs)


def finish(nc, P, dbg_outs):
    P.emit()
    return nc, dbg_outs


def host_consts():
    c = np.zeros((128, K_N), np.float32)
    i = np.arange(128)
    c[:, K_ID:K_ID + 128] = np.eye(128, dtype=np.float32)
    c[:, K_TRI:K_TRI + 128] = (i[:, None] <= i[None, :])
    c[:, K_STRICT:K_STRICT + 128] = (i[:, None] > i[None, :])
    c[:, K_GLA:K_GLA + 128] = (i[:, None] <= i[None, :]) & ((i[:, None] // 64) == (i[None, :] // 64))
    r = np.ones(1024, np.float32)
    r[0::64] = 0.0
    c[:, K_RESET:K_RESET + 1024] = r[None, :]
    return c


def prep_inputs(inp, upto=99):
    f = np.float32
    x = np.ascontiguousarray(inp["x"][0], dtype=f)
    consts = host_consts()
    rows = np.zeros((128, R_NROWS), f)
    rows2 = np.zeros((128, 4096), f)
    rows2[:, 0:2048] = inp["normf_g"][None, :]
    rows2[:, 2048:4096] = inp["ssd_norm_g"][0][None, :]
    rows[:, R_GG:R_GG + 512] = inp["gla_norm_g"][0][None, :]
    rows[:, R_DTB:R_DTB + 32] = inp["dt_bias"][0][None, :]
    rows[:, R_ALOG:R_ALOG + 32] = inp["a_log"][0][None, :]
    rows[:, R_DSK:R_DSK + 32] = inp["d_skip"][0][None, :]
    rows[:, R_RB:R_RB + 64] = inp["router_bias"][0][None, :]
    cols0 = np.zeros((128, C_NCOLS), f)
    cols0[:, C_G1:C_G1 + 16] = inp["norm1_g"][0].reshape(16, 128).T
    cols0[:, C_G2:C_G2 + 16] = inp["norm2_g"][0].reshape(16, 128).T
    cw = inp["conv_w"][0]
    cols0[:, C_CW:C_CW + 96] = cw.reshape(4, 24, 128).transpose(2, 1, 0).reshape(128, 96)
    cols0[:, C_CB:C_CB + 24] = inp["conv_b"][0].reshape(24, 128).T
    cols0[:, C_GB:C_GB + 8] = inp["gla_b_gate"][0].reshape(8, 128).T
    cols0[:, C_CT:C_CT + 16] = inp["c"][0].reshape(16, 128).T
    win = np.ascontiguousarray(inp["w_in"][0], dtype=f)
    wgate = np.ascontiguousarray(inp["gla_w_gate"][0], dtype=f)
    shared = {"consts": consts, "rows": rows, "rows2": rows2}
    if upto >= 1.5:
        shared["win"] = win
        shared["wgate"] = wgate
    if upto >= 4.5:
        shared["wout"] = np.ascontiguousarray(inp["w_out"][0], dtype=f)
        shared["wr"] = np.ascontiguousarray(inp["w_router"][0].reshape(16, 128, NEXP).transpose(1, 0, 2).reshape(128, 16 * NEXP), dtype=f)
    if upto >= 7:
        shared["wsh"] = np.concatenate([inp["w_s_gate"][0], inp["w_s_up"][0], inp["w_s_down"][0].reshape(2048, 512)], axis=0).astype(f)
    maps = []
    for i in range(NCORES):
        m = dict(shared)
        m["x"] = x[i * T:(i + 1) * T]
        m["xh"] = x[i * T - 3:i * T] if i > 0 else np.zeros((3, D), f)
        cl = cols0.copy()
        cl[:, C_FLAG] = 1.0 if i > 0 else 0.0
        for j in range(8):
            cl[:, C_FLAG + 1 + j] = 1.0 if j < i else 0.0
        m["cols"] = cl
        m["wada"] = np.ascontiguousarray(inp["w_ada"][0][:, i * 1536:(i + 1) * 1536], dtype=f)
        m["bada"] = np.ascontiguousarray(inp["b_ada"][0][None, i * 1536:(i + 1) * 1536], dtype=f)
        if upto >= 7:
            parts = []
            for k in range(8):
                ex = 8 * i + k
                parts += [inp["w_e_gate"][0][ex], inp["w_e_up"][0][ex], inp["w_e_down"][0][ex].reshape(2048, 512)]
            m["wexp"] = np.concatenate(parts, axis=0).astype(f)
        maps.append(m)
    return maps


_CACHE = {}


def kernel(**inputs):
    if "nc" not in _CACHE:
        _CACHE["nc"] = build_program()[0]
    nc = _CACHE["nc"]
    maps = prep_inputs(inputs)
    res = run_bass_kernel_spmd(nc, maps, core_ids=list(range(NCORES)))
    out = np.concatenate([r["out"] for r in res.results], axis=0)
    return out.reshape(1, NCORES * T, D).astype(np.float32)
```
